# Optimizing a Trainium2 kernel written in Bass

```python
import jax, jax.numpy as jnp
from jax import lax
import numpy as np

D_MODEL = 2048
BATCH = 2
SEQ = 4096
DEPTH = 4

MLA_HEADS = 8
QK_NOPE_DIM = 128
QK_ROPE_DIM = 64
V_HEAD_DIM = 128
Q_LORA_RANK = 768
KV_LORA_RANK = 512
ROPE_THETA = 10000.0
Q_BLOCK = 128
ATTN_WIDTH = MLA_HEADS * V_HEAD_DIM
SGU_GROUPS = 8
SGU_GROUP_DIM = 128
SGU_WIDTH = SGU_GROUPS * SGU_GROUP_DIM
CHUNK = 128
MIX_WIDTH = ATTN_WIDTH + SGU_WIDTH
SPLIT_AB = (Q_LORA_RANK,
            Q_LORA_RANK + KV_LORA_RANK,
            Q_LORA_RANK + KV_LORA_RANK + QK_ROPE_DIM,
            Q_LORA_RANK + KV_LORA_RANK + QK_ROPE_DIM + SGU_WIDTH)
IN_AB_WIDTH = Q_LORA_RANK + KV_LORA_RANK + QK_ROPE_DIM + 2 * SGU_WIDTH
POOL_WINDOWS = (2, 4, 8, 16)
POOL_GROUP_DIM = D_MODEL // 4
D_FF = 5632
N_EXPERTS = 8
TOP_K = 2
D_FF_EXPERT = 2816
N_EVEN = (DEPTH + 1) // 2
N_ODD = DEPTH // 2
DEEPNORM_ALPHA = (2 * DEPTH) ** 0.25
DEEPNORM_BETA = (8 * DEPTH) ** -0.25
LN_EPS = 1e-5
RMS_EPS = 1e-6

kernel_name = "hybrid_mla_sgu_pool_moe_deepnorm_adaln"


def layer_norm(x, g, b):
    xf = x.astype(jnp.float32)
    mu = jnp.mean(xf, axis=-1, keepdims=True)
    var = jnp.mean(jnp.square(xf - mu), axis=-1, keepdims=True)
    return ((xf - mu) * lax.rsqrt(var + LN_EPS) * g + b).astype(x.dtype)


def rms_norm(x, g):
    xf = x.astype(jnp.float32)
    return (xf * lax.rsqrt(jnp.mean(xf * xf, axis=-1, keepdims=True) + RMS_EPS) * g).astype(x.dtype)


def rope_tables(positions):
    inv = ROPE_THETA ** (-jnp.arange(0, QK_ROPE_DIM, 2, dtype=jnp.float32) / QK_ROPE_DIM)
    ang = positions.astype(jnp.float32)[..., None] * inv
    return jnp.cos(ang), jnp.sin(ang)


def apply_rope(x, cos, sin):
    half = QK_ROPE_DIM // 2
    xf = x.astype(jnp.float32)
    x1, x2 = xf[..., :half], xf[..., half:]
    return jnp.concatenate([x1 * cos - x2 * sin, x1 * sin + x2 * cos], axis=-1).astype(x.dtype)


def ada_modulation(c, w, b):
    mod = jax.nn.silu(c) @ w + b
    shift, scale, gate = jnp.split(mod[:, None, :], 3, axis=-1)
    return shift, scale, gate


def mla_attention(q_nope, q_rope, k_nope, k_rope, v):
    b_, s_, h_, dv = v.shape
    n_blocks = s_ // Q_BLOCK
    sm_scale = (QK_NOPE_DIM + QK_ROPE_DIM) ** -0.5
    k_pos = jnp.arange(s_)

    def block(i):
        start = i * Q_BLOCK
        qn = lax.dynamic_slice_in_dim(q_nope, start, Q_BLOCK, axis=1)
        qr = lax.dynamic_slice_in_dim(q_rope, start, Q_BLOCK, axis=1)
        s = (jnp.einsum('bqhd,bkhd->bhqk', qn, k_nope, preferred_element_type=jnp.float32)
             + jnp.einsum('bqhr,bkr->bhqk', qr, k_rope, preferred_element_type=jnp.float32)) * sm_scale
        causal = (start + jnp.arange(Q_BLOCK))[:, None] >= k_pos[None, :]
        s = jnp.where(causal, s, -jnp.inf)
        p = jax.nn.softmax(s, axis=-1).astype(v.dtype)
        return jnp.einsum('bhqk,bkhd->bqhd', p, v)

    out = lax.map(block, jnp.arange(n_blocks))
    return jnp.moveaxis(out, 0, 1).reshape(b_, s_, h_ * dv)


def spatial_gating(u, v, norm_g, norm_b, w_s, b_s):
    b_, s_, _ = v.shape
    n_chunks = s_ // CHUNK
    vg = v.reshape(b_, s_, SGU_GROUPS, SGU_GROUP_DIM)
    vg = layer_norm(vg, norm_g.reshape(SGU_GROUPS, SGU_GROUP_DIM), norm_b.reshape(SGU_GROUPS, SGU_GROUP_DIM))
    vg = vg.reshape(b_, n_chunks, CHUNK, SGU_GROUPS, SGU_GROUP_DIM)
    w_causal = w_s * jnp.tril(jnp.ones((CHUNK, CHUNK), dtype=w_s.dtype))
    s = jnp.einsum('gts,bnsgc->bntgc', w_causal, vg) + b_s.T[None, None, :, :, None]
    return u * s.reshape(b_, s_, SGU_WIDTH)


def mixer_ab(h, cos, sin, w_in, q_norm_g, w_q_up, kv_norm_g, w_kv_up,
             sgu_norm_g, sgu_norm_b, sgu_w, sgu_b, w_out):
    b_, s_, _ = h.shape
    z = h @ w_in
    cq, ckv, kr, zu, zv = jnp.split(z, SPLIT_AB, axis=-1)
    q = (rms_norm(cq, q_norm_g) @ w_q_up).reshape(b_, s_, MLA_HEADS, QK_NOPE_DIM + QK_ROPE_DIM)
    q_nope = q[..., :QK_NOPE_DIM]
    q_rope = apply_rope(q[..., QK_NOPE_DIM:], cos[:, :, None, :], sin[:, :, None, :])
    kv = (rms_norm(ckv, kv_norm_g) @ w_kv_up).reshape(b_, s_, MLA_HEADS, QK_NOPE_DIM + V_HEAD_DIM)
    k_nope, v = kv[..., :QK_NOPE_DIM], kv[..., QK_NOPE_DIM:]
    k_rope = apply_rope(kr, cos, sin)
    attn = mla_attention(q_nope, q_rope, k_nope, k_rope, v)
    sgu = spatial_gating(jax.nn.gelu(zu), jax.nn.gelu(zv), sgu_norm_g, sgu_norm_b, sgu_w, sgu_b)
    return jnp.concatenate([attn, sgu], axis=-1) @ w_out


def multiscale_pool(h, w_pool, pool_scale):
    s_ = h.shape[1]
    hf = h.astype(jnp.float32)
    cs0 = jnp.pad(jnp.cumsum(hf, axis=1), ((0, 0), (1, 0), (0, 0)))
    count_base = jnp.arange(1, s_ + 1, dtype=jnp.float32)[None, :, None]
    outs = []
    for gi, w in enumerate(POOL_WINDOWS):
        sl = slice(gi * POOL_GROUP_DIM, (gi + 1) * POOL_GROUP_DIM)
        cg = cs0[:, :, sl]
        lag = jnp.pad(cg, ((0, 0), (w, 0), (0, 0)))[:, 1:s_ + 1]
        mean = (cg[:, 1:] - lag) / jnp.minimum(count_base, float(w))
        d = (mean - hf[:, :, sl]).astype(h.dtype)
        outs.append(d @ w_pool[gi])
    return jnp.concatenate(outs, axis=-1) * pool_scale


def swiglu(h, w_gate, w_up, w_down):
    return (jax.nn.silu(h @ w_gate) * (h @ w_up)) @ w_down


def moe_swiglu(h, w_router, w_gate, w_up, w_down):
    b_, s_, d_ = h.shape
    t = h.reshape(-1, d_)
    logits = jnp.dot(t, w_router, preferred_element_type=jnp.float32)
    top_v, top_i = lax.top_k(logits, TOP_K)
    top_w = jax.nn.softmax(top_v, axis=-1)
    gates = jnp.sum(jax.nn.one_hot(top_i, N_EXPERTS, dtype=jnp.float32) * top_w[..., None], axis=1)
    gates = gates.astype(t.dtype)
    y = jnp.zeros_like(t)
    for e in range(N_EXPERTS):
        y = y + gates[:, e:e + 1] * swiglu(t, w_gate[e], w_up[e], w_down[e])
    return y.reshape(b_, s_, d_)


def setup_inputs(seed: int = 0) -> dict:
    key = jax.random.key(seed)
    ks = jax.random.split(key, 32)
    f32 = jnp.float32

    def nrm(k, shape, scale):
        return jax.random.normal(k, shape, f32) * scale

    D = D_MODEL
    offset = jax.random.randint(ks[2], (BATCH, 1), 0, 1024, dtype=jnp.int32)
    positions = (offset + jnp.arange(SEQ, dtype=jnp.int32)[None, :]).astype(jnp.int32)
    return {
        "x": nrm(ks[0], (BATCH, SEQ, D), 1.0),
        "c": nrm(ks[1], (BATCH, D), 1.0),
        "positions": positions,
        "ada_w": nrm(ks[3], (DEPTH, 2, D, 3 * D), 0.1 * D ** -0.5),
        "ada_b": nrm(ks[4], (DEPTH, 2, 3 * D), 0.02),
        "ln_g": 1.0 + nrm(ks[5], (DEPTH, 2, D), 0.02),
        "ln_b": nrm(ks[6], (DEPTH, 2, D), 0.02),
        "w_in_ab": nrm(ks[7], (N_EVEN, D, IN_AB_WIDTH), D ** -0.5),
        "q_norm_g": 1.0 + nrm(ks[8], (N_EVEN, Q_LORA_RANK), 0.02),
        "w_q_up": nrm(ks[9], (N_EVEN, Q_LORA_RANK, MLA_HEADS * (QK_NOPE_DIM + QK_ROPE_DIM)), Q_LORA_RANK ** -0.5),
        "kv_norm_g": 1.0 + nrm(ks[10], (N_EVEN, KV_LORA_RANK), 0.02),
        "w_kv_up": nrm(ks[11], (N_EVEN, KV_LORA_RANK, MLA_HEADS * (QK_NOPE_DIM + V_HEAD_DIM)), KV_LORA_RANK ** -0.5),
        "sgu_norm_g": 1.0 + nrm(ks[12], (N_EVEN, SGU_WIDTH), 0.02),
        "sgu_norm_b": nrm(ks[13], (N_EVEN, SGU_WIDTH), 0.02),
        "sgu_w": nrm(ks[14], (N_EVEN, SGU_GROUPS, CHUNK, CHUNK), CHUNK ** -0.5),
        "sgu_b": 1.0 + nrm(ks[15], (N_EVEN, SGU_GROUPS, CHUNK), 0.02),
        "w_out_ab": nrm(ks[16], (N_EVEN, MIX_WIDTH, D), DEEPNORM_BETA * MIX_WIDTH ** -0.5),
        "ffn_w_gate": nrm(ks[17], (N_EVEN, D, D_FF), D ** -0.5),
        "ffn_w_up": nrm(ks[18], (N_EVEN, D, D_FF), D ** -0.5),
        "ffn_w_down": nrm(ks[19], (N_EVEN, D_FF, D), DEEPNORM_BETA * D_FF ** -0.5),
        "pool_w": nrm(ks[20], (N_ODD, len(POOL_WINDOWS), POOL_GROUP_DIM, POOL_GROUP_DIM), POOL_GROUP_DIM ** -0.5),
        "pool_scale": 1.0 + nrm(ks[21], (N_ODD, D), 0.02),
        "w_out_c": nrm(ks[22], (N_ODD, D, D), DEEPNORM_BETA * D ** -0.5),
        "router_w": nrm(ks[23], (N_ODD, D, N_EXPERTS), D ** -0.5),
        "moe_w_gate": nrm(ks[24], (N_ODD, N_EXPERTS, D, D_FF_EXPERT), D ** -0.5),
        "moe_w_up": nrm(ks[25], (N_ODD, N_EXPERTS, D, D_FF_EXPERT), D ** -0.5),
        "moe_w_down": nrm(ks[26], (N_ODD, N_EXPERTS, D_FF_EXPERT, D), DEEPNORM_BETA * D_FF_EXPERT ** -0.5),
    }


def reference(x, c, positions, ada_w, ada_b, ln_g, ln_b, w_in_ab, q_norm_g, w_q_up,
              kv_norm_g, w_kv_up, sgu_norm_g, sgu_norm_b, sgu_w, sgu_b, w_out_ab,
              ffn_w_gate, ffn_w_up, ffn_w_down, pool_w, pool_scale, w_out_c, router_w,
              moe_w_gate, moe_w_up, moe_w_down):
    cos, sin = rope_tables(positions)
    for l in range(DEPTH):
        j = l // 2
        shift, scale, gate = ada_modulation(c, ada_w[l, 0], ada_b[l, 0])
        hm = x * (1 + scale) + shift
        if l % 2 == 0:
            y = mixer_ab(hm, cos, sin, w_in_ab[j], q_norm_g[j], w_q_up[j], kv_norm_g[j], w_kv_up[j],
                         sgu_norm_g[j], sgu_norm_b[j], sgu_w[j], sgu_b[j], w_out_ab[j])
        else:
            y = multiscale_pool(hm, pool_w[j], pool_scale[j]) @ w_out_c[j]
        x = layer_norm(DEEPNORM_ALPHA * x + (1 + gate) * y, ln_g[l, 0], ln_b[l, 0])
        shift, scale, gate = ada_modulation(c, ada_w[l, 1], ada_b[l, 1])
        hm = x * (1 + scale) + shift
        if l % 2 == 0:
            y = swiglu(hm, ffn_w_gate[j], ffn_w_up[j], ffn_w_down[j])
        else:
            y = moe_swiglu(hm, router_w[j], moe_w_gate[j], moe_w_up[j], moe_w_down[j])
        x = layer_norm(DEEPNORM_ALPHA * x + (1 + gate) * y, ln_g[l, 1], ln_b[l, 1])
    return x
```

```python
import math
import numpy as np
from contextlib import ExitStack
import concourse.bass as bass
import concourse.mybir as mybir
from concourse.bass_utils import run_bass_kernel_spmd

F32 = mybir.dt.float32
BF16 = mybir.dt.bfloat16
I32 = mybir.dt.int32
AF = mybir.ActivationFunctionType
ALU = mybir.AluOpType
AX = mybir.AxisListType

D = 2048
KC = 16
T = 1024
NB = 8
DEPTH = 4
ALPHA = (2 * DEPTH) ** 0.25
LN_EPS = 1e-5
RMS_EPS = 1e-6
QR = 768
KVR = 512
NH = 8
SM_SCALE = 192 ** -0.5
DFF = 5632
DFE = 2816
NE = 8
W_IN_EXT = 3456


class Buf:
    __slots__ = ("name", "lw", "rd", "excl")

    def __init__(self, name, excl=False):
        self.name = name
        self.lw = None
        self.rd = []
        self.excl = excl


class Chan:
    def __init__(self, sem):
        self.sem = sem
        self.count = 0
        self.last = None


class Op:
    __slots__ = ("eng", "fn", "deps", "signal", "count", "chan", "chan_idx", "is_dma", "flushed")

    def __init__(self, eng, fn):
        self.eng = eng
        self.fn = fn
        self.deps = []
        self.signal = False
        self.count = 0
        self.chan = None
        self.chan_idx = 0
        self.is_dma = False
        self.flushed = False


class Prog:
    ENGS = ("pe", "act", "dve", "pool", "sp")

    def __init__(self, nc, stack):
        self.nc = nc
        self.root = stack
        self.stack = stack
        self.q = {e: [] for e in self.ENGS}
        self.sems = {e: stack.enter_context(nc.semaphore("sem_" + e)) for e in self.ENGS}
        self.cnt = {e: 0 for e in self.ENGS}
        self.waited = {e: {} for e in self.ENGS}
        self.free_ch = []
        self.used_ch = []
        self.nchan = 0
        self.nt = 0
        self.tag = "g"

    def stage(self, tag):
        return _Stage(self, tag)

    def sb(self, shape, dt, name=None):
        self.nt += 1
        return self.stack.enter_context(
            self.nc.sbuf_tensor("s_%s_%s%d" % (self.tag, name or "t", self.nt), list(shape), dt))

    def ps(self, shape, dt=F32, name=None):
        self.nt += 1
        return self.stack.enter_context(
            self.nc.psum_tensor("p_%s_%s%d" % (self.tag, name or "t", self.nt), list(shape), dt))

    def buf(self, name=None):
        return Buf(name)

    def bufs(self, n):
        return [Buf(None) for _ in range(n)]

    def chan(self):
        if self.free_ch:
            c = self.free_ch.pop()
        else:
            self.nchan += 1
            c = Chan(self.root.enter_context(self.nc.semaphore("ch%d" % self.nchan)))
        self.used_ch.append(c)
        return c

    def _track(self, op, reads, writes):
        ex = [b for b in reads if b.excl]
        if ex:
            reads = [b for b in reads if not b.excl]
            writes = list(writes) + ex
        deps = []
        for b in reads:
            if b.lw is not None:
                deps.append(b.lw)
        for b in writes:
            if b.lw is not None:
                deps.append(b.lw)
            deps.extend(b.rd)
        for b in reads:
            b.rd.append(op)
        for b in writes:
            b.lw = op
            b.rd = []
        seen = set()
        for d in deps:
            if d is op or id(d) in seen or d.flushed:
                continue
            seen.add(id(d))
            op.deps.append(d)

    def op(self, eng, fn, reads=(), writes=()):
        o = Op(eng, fn)
        self._track(o, reads, writes)
        self.q[eng].append(o)
        return o

    def dma(self, eng, chan, out, in_, reads=(), writes=(), **kw):
        def fn(e):
            return e.dma_start(out=out, in_=in_, **kw)
        o = Op(eng, fn)
        o.is_dma = True
        o.chan = chan
        self._track(o, reads, writes)
        if chan.last is not None and not chan.last.flushed:
            o.deps.append(chan.last)
        chan.count += 1
        o.chan_idx = chan.count
        chan.last = o
        self.q[eng].append(o)
        return o

    def flush(self):
        nc = self.nc
        for e in self.ENGS:
            for o in self.q[e]:
                nd = []
                for d in o.deps:
                    if d.flushed:
                        continue
                    if d.is_dma:
                        nd.append(d)
                        continue
                    if d.eng == o.eng and not o.is_dma and o.eng == "pe":
                        continue
                    d.signal = True
                    nd.append(d)
                o.deps = nd
        lasts = []
        for e in self.ENGS:
            comp = [o for o in self.q[e] if not o.is_dma]
            if comp:
                comp[-1].signal = True
                lasts.append(comp[-1])
        chans = {}
        for e in self.ENGS:
            for o in self.q[e]:
                if o.is_dma:
                    chans[id(o.chan)] = o.chan.last
        lasts.extend(chans.values())
        for e in self.ENGS:
            for o in self.q[e]:
                if not o.is_dma and o.signal:
                    self.cnt[e] += 1
                    o.count = self.cnt[e]

        def run(ename, eng):
            waited = self.waited[ename]

            def wait_deps(deps):
                need = {}
                for d in deps:
                    if d.is_dma:
                        sem, val = d.chan.sem, 16 * d.chan_idx
                    else:
                        sem, val = self.sems[d.eng], d.count
                    if need.get(id(sem), (None, 0))[1] < val:
                        need[id(sem)] = (sem, val)
                for sem, val in need.values():
                    if waited.get(id(sem), 0) >= val:
                        continue
                    waited[id(sem)] = val
                    eng.wait_ge(sem, val)
            for o in self.q[ename]:
                wait_deps(o.deps)
                ins = o.fn(eng)
                if o.is_dma:
                    ins.then_inc(o.chan.sem, 16)
                elif o.signal:
                    ins.then_inc(self.sems[ename], 1)
            wait_deps(lasts)

        with nc.Block() as block:
            @block.tensor
            def _(eng):
                run("pe", eng)

            @block.scalar
            def _(eng):
                run("act", eng)

            @block.vector
            def _(eng):
                run("dve", eng)

            @block.gpsimd
            def _(eng):
                run("pool", eng)

            @block.sync
            def _(eng):
                run("sp", eng)
        for e in self.ENGS:
            for o in self.q[e]:
                o.flushed = True
                o.fn = None
            self.q[e] = []
        self.free_ch.extend(self.used_ch)
        self.used_ch = []


class _Stage:
    def __init__(self, P, tag):
        self.P = P
        self.tag = tag

    def __enter__(self):
        self.prev = (self.P.stack, self.P.tag)
        self.st = ExitStack()
        self.st.__enter__()
        self.P.stack = self.st
        self.P.tag = self.tag
        return self.P

    def __exit__(self, *a):
        if a[0] is None:
            self.P.flush()
        self.P.stack, self.P.tag = self.prev
        return self.st.__exit__(*a)


class WStream:
    def __init__(self, P, nslots, kc, cols, name="w"):
        self.P = P
        self.tiles = [P.sb([128, kc, cols], BF16, name=name) for _ in range(nslots)]
        self.bufs = P.bufs(nslots)
        self.chans = [P.chan() for _ in range(nslots)]
        self.i = 0
        self.n = nslots

    def load(self, src, kc, cols, eng="pool"):
        s = self.i % self.n
        self.i += 1
        view = self.tiles[s][:, 0:kc, 0:cols]
        self.P.dma(eng, self.chans[s], view, src.rearrange("(k p) n -> p k n", p=128), writes=[self.bufs[s]])
        return view, self.bufs[s]


class Banks:
    def __init__(self, P, n=8):
        self.t = [P.ps([128, 512], F32, name="bank") for _ in range(n)]
        self.b = [Buf(None, excl=True) for _ in range(n)]
        self.i = 0
        self.n = n

    def next(self, lo=0, hi=None):
        hi = self.n if hi is None else hi
        k = lo + self.i % (hi - lo)
        self.i += 1
        return self.t[k], self.b[k]


DBG = {}
CV = {}


def rsqrt_op(P, out, in_, bias_col, scale, reads, writes):
    t = CV["t"]
    P.op("act", lambda e: e.activation(out, in_, AF.Sqrt, bias=t[0:out.shape[0], bias_col:bias_col + 1], scale=scale),
         reads=list(reads), writes=list(writes))
    P.op("dve", lambda e: e.reciprocal(out, out), reads=list(writes), writes=list(writes))


def mm(P, out, outb, lhsT, rhs, reads, start, stop):
    P.op("pe", lambda e: e.matmul(out, lhsT=lhsT, rhs=rhs, start=start, stop=stop), reads=reads, writes=[outb])


def stage_ada(P, tag, ccol, adaw, adab_row, adab_col, want_col, want_bc, out_col, out_bc):
    with P.stage(tag):
        ws = WStream(P, 3, KC, 512)
        bk = Banks(P, 6)
        ch = P.chan()
        c_sb = P.sb([128, KC], F32)
        sc_f = P.sb([128, KC], F32)
        sc_bf = P.sb([128, KC], BF16)
        sc_rep = P.sb([128, KC, 128], BF16)
        bcol = P.sb([128, 48], F32)
        brow = [P.sb([128, 512], F32) for _ in range(2)]
        browb = P.bufs(2)
        browc = [P.chan() for _ in range(2)]
        cb, scb, colb, ocb, obb = P.bufs(5)
        P.dma("sp", ch, c_sb[:], ccol, writes=[cb])
        P.dma("sp", ch, bcol[:], adab_col, writes=[colb])
        P.op("act", lambda e: e.activation(sc_f[:], c_sb[:], AF.Silu), reads=[cb], writes=[scb])
        P.op("dve", lambda e: e.tensor_copy(sc_bf[:], sc_f[:]), reads=[scb], writes=[scb])
        for k in range(KC):
            P.op("dve", lambda e, k=k: e.tensor_copy(sc_rep[:, k, :], sc_f[:, k:k + 1].to_broadcast([128, 128])),
                 reads=[scb], writes=[scb])
        for q in range(12):
            third, off = q // 4, (q % 4) * 512
            wt, wb = ws.load(adaw[:, 512 * q:512 * (q + 1)], KC, 512)
            add1 = 1.0 if third > 0 else 0.0
            if third in want_bc:
                r = q % 2
                P.dma("sp", browc[r], brow[r][:], adab_row[0:1, 512 * q:512 * (q + 1)].to_broadcast([128, 512]),
                      writes=[browb[r]])
                pt, pb = bk.next(0, 4)
                for k in range(KC):
                    mm(P, pt[:], pb, sc_rep[:, k, :], wt[:, k, :], [scb, wb], k == 0, k == KC - 1)
                dst = out_bc[third][:, off:off + 512]
                P.op("dve", lambda e, pt=pt, r=r, dst=dst, add1=add1: e.scalar_tensor_tensor(
                    out=dst, in0=pt[:], scalar=add1, in1=brow[r][:], op0=ALU.add, op1=ALU.add),
                    reads=[pb, browb[r]], writes=[obb])
            if third in want_col:
                pt, pb = bk.next(4, 6)
                for m in range(4):
                    for k in range(KC):
                        mm(P, pt[:, m:m + 1], pb, wt[:, k, 128 * m:128 * (m + 1)], sc_bf[:, k:k + 1], [scb, wb],
                           k == 0, k == KC - 1)
                j0 = (q % 4) * 4
                dst = out_col[third][:, j0:j0 + 4]
                P.op("dve", lambda e, pt=pt, dst=dst, add1=add1, q=q: e.scalar_tensor_tensor(
                    out=dst, in0=pt[:, 0:4], scalar=add1, in1=bcol[:, 4 * q:4 * q + 4], op0=ALU.add, op1=ALU.add),
                    reads=[pb, colb], writes=[ocb])


def stage_hmt(P, tag, xsrc, ident, scc, shc, hmT, router=None):
    with P.stage(tag):
        bk = Banks(P, 6)
        x_sb = P.sb([128, NB, D], F32, name="x")
        xb = P.bufs(NB)
        hb = P.buf()
        chx = [P.chan() for _ in range(2)]
        for b in range(NB):
            P.dma("sp", chx[b % 2], x_sb[:, b, :], xsrc[128 * b:128 * (b + 1), :], writes=[xb[b]])
        if router is not None:
            ch = P.chan()
            wr_sb = P.sb([128, KC, NE], F32)
            wrb = P.buf()
            P.dma("sp", ch, wr_sb[:], router["wr"].rearrange("(k p) e -> p k e", p=128), writes=[wrb])
            h32 = [P.sb([128, 512], F32) for _ in range(2)]
            h32b = P.bufs(2)
            lps = [P.ps([NE, 512], F32) for _ in range(2)]
            lpb = [Buf(None, excl=True) for _ in range(2)]
        n = 0
        for th in range(2):
            for kc in range(KC):
                pt, pb = bk.next()
                for i in range(4):
                    b = 4 * th + i
                    P.op("pe", lambda e, pt=pt, i=i, b=b, kc=kc: e.transpose(
                        pt[:, 128 * i:128 * (i + 1)], x_sb[:, b, 128 * kc:128 * (kc + 1)], ident[:]),
                        reads=[xb[b]], writes=[pb])
                P.op("act", lambda e, pt=pt, kc=kc, th=th: e.activation(
                    hmT[:, kc, 512 * th:512 * (th + 1)], pt[:], AF.Identity,
                    bias=shc[:, kc:kc + 1], scale=scc[:, kc:kc + 1]), reads=[pb], writes=[hb])
                if router is not None:
                    r = n % 2
                    n += 1
                    P.op("dve", lambda e, pt=pt, kc=kc, r=r: e.tensor_scalar(
                        h32[r][:], pt[:], scc[:, kc:kc + 1], shc[:, kc:kc + 1], ALU.mult, ALU.add),
                        reads=[pb], writes=[h32b[r]])
                    P.op("pe", lambda e, kc=kc, r=r, th=th: e.matmul(
                        lps[th][:], lhsT=wr_sb[:, kc, :], rhs=h32[r][:], start=(kc == 0), stop=(kc == KC - 1)),
                        reads=[h32b[r], wrb], writes=[lpb[th]])
            if router is not None:
                P.op("dve", lambda e, th=th: e.tensor_copy(router["logT"][:, 512 * th:512 * (th + 1)], lps[th][:]),
                     reads=[lpb[th]], writes=[hb])


def rope_tables(P, pos_bc, rc, cos_t, sin_t):
    ch = P.chan()
    pi_ = P.sb([64, T], I32)
    pf = P.sb([64, T], F32)
    y = P.sb([64, T], F32)
    u = P.sb([64, T], F32)
    ki = P.sb([64, T], I32)
    pb_, fb, tb = P.bufs(3)
    TWO_PI = 2 * math.pi
    P.dma("sp", ch, pi_[:], pos_bc, writes=[pb_])
    P.op("dve", lambda e: e.tensor_copy(pf[:], pi_[:]), reads=[pb_], writes=[fb])
    P.op("dve", lambda e: e.tensor_scalar(pf[:], pf[:], rc[:, 0:1], None, ALU.mult), reads=[fb], writes=[fb])
    for col, dst in ((1, cos_t), (2, sin_t)):
        P.op("dve", lambda e, col=col: e.tensor_scalar(y[:], pf[:], rc[:, col:col + 1], None, ALU.add), reads=[fb], writes=[fb])
        P.op("dve", lambda e: e.tensor_scalar(u[:], y[:], 1.0 / TWO_PI, None, ALU.mult), reads=[fb], writes=[fb])
        P.op("dve", lambda e: e.tensor_copy(ki[:], u[:]), reads=[fb], writes=[fb])
        P.op("dve", lambda e: e.tensor_copy(u[:], ki[:]), reads=[fb], writes=[fb])
        P.op("dve", lambda e: e.scalar_tensor_tensor(out=y[:], in0=u[:], scalar=-TWO_PI, in1=y[:], op0=ALU.mult, op1=ALU.add),
             reads=[fb], writes=[fb])
        P.op("dve", lambda e: e.tensor_scalar(u[:], y[:], math.pi, -TWO_PI, ALU.is_gt, ALU.mult), reads=[fb], writes=[fb])
        P.op("dve", lambda e: e.tensor_tensor(y[:], y[:], u[:], ALU.add), reads=[fb], writes=[fb])
        P.op("dve", lambda e: e.tensor_scalar(u[:], y[:], -math.pi, TWO_PI, ALU.is_lt, ALU.mult), reads=[fb], writes=[fb])
        P.op("dve", lambda e: e.tensor_tensor(y[:], y[:], u[:], ALU.add), reads=[fb], writes=[fb])
        P.op("dve", lambda e: e.tensor_scalar(y[:], y[:], math.pi, -math.pi, ALU.min, ALU.max), reads=[fb], writes=[fb])
        P.op("act", lambda e, dst=dst: e.activation(dst[:], y[:], AF.Sin), reads=[fb], writes=[tb])
    return tb


def rms_chunks(P, bk, ws_tiles, hmT, hb, cols, nchunk, gcol, dst, dstb, ones_bf, ssbank, ssb, sq, sqb):
    pend = []
    n = 0
    for m in range(nchunk):
        wt, wb, co = ws_tiles(m)
        for th in range(2):
            pt, pb = bk.next(0, 4)
            for k in range(KC):
                mm(P, pt[:], pb, wt[:, k, co:co + 128], hmT[:, k, 512 * th:512 * (th + 1)], [wb, hb], k == 0, k == KC - 1)
            for f in pend:
                f()
            pend = []
            r = n % 2
            n += 1
            if DBG.get("sq") == "dve":
                P.op("dve", lambda e, pt=pt, r=r: e.tensor_tensor(sq[r][:], pt[:], pt[:], ALU.mult), reads=[pb], writes=[sqb[r]])
            elif DBG.get("sq") == "copy":
                P.op("act", lambda e, pt=pt, r=r: e.activation(sq[r][:], pt[:], AF.Identity), reads=[pb], writes=[sqb[r]])
            else:
                P.op("act", lambda e, pt=pt, r=r: e.activation(sq[r][:], pt[:], AF.Square), reads=[pb], writes=[sqb[r]])
            P.op("dve", lambda e, pt=pt, m=m, th=th: e.tensor_scalar(
                dst[:, m, 512 * th:512 * (th + 1)], pt[:], gcol[:, m:m + 1], None, ALU.mult), reads=[pb], writes=[dstb])

            def later(r=r, th=th, m=m):
                mm(P, ssbank[th][:], ssb[th], ones_bf[:], sq[r][:], [sqb[r]], m == 0, m == nchunk - 1)
            pend.append(later)
    for f in pend:
        f()


def rstd_from_ss(P, ssbank, ssb, n, out, outb, post=1.0):
    for th in range(2):
        o = out[:, 512 * th:512 * (th + 1)]
        rsqrt_op(P, o, ssbank[th][:], 1 if post == 1.0 else 2, 1.0 / (n * post * post), [ssb[th]], [outb])


def latent_part(P, bk, ws, w_in, hmT, hb, kvg, ones_bf, cos_t, sin_t, tb, lat, latb, sq, sqb):
    ssbank = [bk.t[4], bk.t[5]]
    ssb = [bk.b[4], bk.b[5]]
    tiles = {}

    def wt_of(m):
        col = 768 + 128 * m
        t0 = col // 512
        if t0 not in tiles:
            tiles[t0] = ws.load(w_in[:, 512 * t0:512 * t0 + 512], KC, 512)
        return tiles[t0][0], tiles[t0][1], col - 512 * t0
    if DBG.get("stop") == "pre":
        return
    rms_chunks(P, bk, wt_of, hmT, hb, None, 4, kvg, lat, latb, ones_bf, ssbank, ssb, sq, sqb)
    if DBG.get("stop") == "rms":
        return
    rstd = P.sb([128, T], F32)
    rb = P.buf()
    rstd_from_ss(P, ssbank, ssb, KVR, rstd, rb)
    for m in range(4):
        P.op("dve", lambda e, m=m: e.tensor_tensor(lat[:, m, :], lat[:, m, :], rstd[:], ALU.mult),
             reads=[rb, latb], writes=[latb])
    if DBG.get("stop") == "rstd":
        return
    wt, wb = tiles[2] if 2 in tiles else ws.load(w_in[:, 1024:1536], KC, 512)
    t1 = P.sb([64, 512], F32)
    t2 = P.sb([64, 512], F32)
    t1b, t2b = P.bufs(2)
    for th in range(2):
        pa, pab = bk.next(0, 4)
        pbk, pbb = bk.next(0, 4)
        for k in range(KC):
            mm(P, pa[0:64, :], pab, wt[:, k, 256:320], hmT[:, k, 512 * th:512 * (th + 1)], [wb, hb], k == 0, k == KC - 1)
        for k in range(KC):
            mm(P, pbk[0:64, :], pbb, wt[:, k, 320:384], hmT[:, k, 512 * th:512 * (th + 1)], [wb, hb], k == 0, k == KC - 1)
        sl = slice(512 * th, 512 * (th + 1))
        P.op("dve", lambda e, pa=pa, sl=sl: e.tensor_tensor(t1[:], pa[0:64, :], cos_t[:, sl], ALU.mult),
             reads=[pab, tb], writes=[t1b])
        P.op("dve", lambda e, pbk=pbk, sl=sl: e.tensor_tensor(t2[:], pbk[0:64, :], sin_t[:, sl], ALU.mult),
             reads=[pbb, tb], writes=[t2b])
        P.op("dve", lambda e, sl=sl: e.tensor_tensor(lat[0:64, 4, sl], t1[:], t2[:], ALU.add),
             reads=[t1b, t2b], writes=[latb])


def ln_blocks(P, x_sb, xb, lng, lnb, gb_buf, outdst, och):
    stats = P.sb([128, NB, 4, 6], F32)
    mv = P.sb([128, NB, 2], F32)
    rstd = P.sb([128, NB], F32)
    nmr = P.sb([128, NB], F32)
    sb_ = P.buf()
    outs = []
    for b in range(NB):
        for c4 in range(4):
            P.op("dve", lambda e, b=b, c4=c4: e.bn_stats(stats[:, b, c4, :], x_sb[:, b, 512 * c4:512 * (c4 + 1)]),
                 reads=[xb[b]], writes=[sb_])
        P.op("dve", lambda e, b=b: e.bn_aggr(mv[:, b, :], stats[:, b, :, :].rearrange("p a s -> p (a s)")),
             reads=[sb_], writes=[sb_])
        rsqrt_op(P, rstd[:, b:b + 1], mv[:, b, 1:2], 0, 1.0, [sb_], [sb_])
        P.op("dve", lambda e, b=b: e.scalar_tensor_tensor(out=nmr[:, b:b + 1], in0=mv[:, b, 0:1], scalar=-1.0,
                                                          in1=rstd[:, b:b + 1], op0=ALU.mult, op1=ALU.mult),
             reads=[sb_], writes=[sb_])
        P.op("act", lambda e, b=b: e.activation(x_sb[:, b, :], x_sb[:, b, :], AF.Identity,
                                                bias=nmr[:, b:b + 1], scale=rstd[:, b:b + 1]),
             reads=[sb_, xb[b]], writes=[xb[b]])
        P.op("dve", lambda e, b=b: e.tensor_tensor(x_sb[:, b, :], x_sb[:, b, :], lng[:], ALU.mult),
             reads=[xb[b], gb_buf], writes=[xb[b]])
        P.op("dve", lambda e, b=b: e.tensor_tensor(x_sb[:, b, :], x_sb[:, b, :], lnb[:], ALU.add),
             reads=[xb[b], gb_buf], writes=[xb[b]])
        outs.append(P.dma("sp", och[b % 2], outdst[128 * b:128 * (b + 1), :], x_sb[:, b, :], reads=[xb[b]]))
    return outs


def load_ln(P, lng_d, lnb_d, lng, lnb, gb_buf):
    ch = P.chan()
    P.dma("sp", ch, lng[:], lng_d[0:1, :].to_broadcast([128, D]), writes=[gb_buf])
    P.dma("sp", ch, lnb[:], lnb_d[0:1, :].to_broadcast([128, D]), writes=[gb_buf])


def out_proj_ln(P, tag, srcT, w_d, xsrc, xdst, G, lng_d, lnb_d):
    with P.stage(tag):
        ws = WStream(P, 2, KC, 512)
        bk = Banks(P, 8)
        x_sb = P.sb([128, NB, D], F32, name="x")
        xb = P.bufs(NB)
        lng = P.sb([128, D], F32)
        lnb = P.sb([128, D], F32)
        gb_buf = P.buf()
        load_ln(P, lng_d, lnb_d, lng, lnb, gb_buf)
        chx = [P.chan() for _ in range(2)]
        tmp = [P.sb([128, 512], F32) for _ in range(2)]
        tmpb = P.bufs(2)
        sb = P.buf()
        for b in range(NB):
            P.dma("sp", chx[b % 2], x_sb[:, b, :], xsrc[128 * b:128 * (b + 1), :], writes=[xb[b]])
        n = 0
        for ft in range(4):
            wt, wb = ws.load(w_d[:, 512 * ft:512 * (ft + 1)], KC, 512)
            for b in range(NB):
                pt, pb = bk.next()
                for k in range(KC):
                    mm(P, pt[:], pb, srcT[:, k, 128 * b:128 * (b + 1)], wt[:, k, :], [wb, sb], k == 0, k == KC - 1)
                r = n % 2
                n += 1
                fs = slice(512 * ft, 512 * (ft + 1))
                P.op("dve", lambda e, pt=pt, r=r, fs=fs: e.tensor_tensor(tmp[r][:], pt[:], G[:, fs], ALU.mult),
                     reads=[pb], writes=[tmpb[r]])
                P.op("dve", lambda e, r=r, b=b, fs=fs: e.scalar_tensor_tensor(
                    out=x_sb[:, b, fs], in0=x_sb[:, b, fs], scalar=ALPHA, in1=tmp[r][:], op0=ALU.mult, op1=ALU.add),
                    reads=[tmpb[r], xb[b]], writes=[xb[b]])
        ln_blocks(P, x_sb, xb, lng, lnb, gb_buf, xdst, chx)


def ffn_stage(P, tag, xsrc, xdst, ident, scc, shc, G, lng_d, lnb_d, experts, router_w=None):
    with P.stage(tag + "o"):
        x_sb = P.sb([128, NB, D], F32, name="x")
        xb = P.bufs(NB)
        chx = [P.chan() for _ in range(2)]
        with P.stage(tag + "e"):
            _ffn_experts(P, xsrc, ident, scc, shc, G, experts, router_w, x_sb, xb, chx)
        with P.stage(tag + "n"):
            lng = P.sb([128, D], F32)
            lnb = P.sb([128, D], F32)
            gb_buf = P.buf()
            load_ln(P, lng_d, lnb_d, lng, lnb, gb_buf)
            ln_blocks(P, x_sb, xb, lng, lnb, gb_buf, xdst, chx)


def _ffn_experts(P, xsrc, ident, scc, shc, G, experts, router_w, x_sb, xb, chx):
    if True:
        hmT = P.sb([128, KC, T], BF16, name="hmT")
        hb = P.buf()
        moe = router_w is not None
        for b in range(NB):
            P.dma("sp", chx[b % 2], x_sb[:, b, :], xsrc[128 * b:128 * (b + 1), :], writes=[xb[b]])
        bk = Banks(P, 8)
        if moe:
            chw = P.chan()
            wr_sb = P.sb([128, KC, NE], F32)
            wrb = P.buf()
            P.dma("sp", chw, wr_sb[:], router_w.rearrange("(k p) e -> p k e", p=128), writes=[wrb])
            h32 = [P.sb([128, 512], F32) for _ in range(2)]
            h32b = P.bufs(2)
            logT = P.sb([NE, T], F32)
            lgb = P.buf()
            gates = P.sb([128, NB, NE], F32)
            gtb = P.buf()
        n = 0
        for th in range(2):
            lp, lpb = bk.t[6 + th], bk.b[6 + th]
            for kc in range(KC):
                pt, pb = bk.next(0, 6)
                for i in range(4):
                    b = 4 * th + i
                    P.op("pe", lambda e, pt=pt, i=i, b=b, kc=kc: e.transpose(
                        pt[:, 128 * i:128 * (i + 1)], x_sb[:, b, 128 * kc:128 * (kc + 1)], ident[:]),
                        reads=[xb[b]], writes=[pb])
                P.op("act", lambda e, pt=pt, kc=kc, th=th: e.activation(
                    hmT[:, kc, 512 * th:512 * (th + 1)], pt[:], AF.Identity,
                    bias=shc[:, kc:kc + 1], scale=scc[:, kc:kc + 1]), reads=[pb], writes=[hb])
                if moe:
                    r = n % 2
                    n += 1
                    P.op("dve", lambda e, pt=pt, kc=kc, r=r: e.tensor_scalar(
                        h32[r][:], pt[:], scc[:, kc:kc + 1], shc[:, kc:kc + 1], ALU.mult, ALU.add),
                        reads=[pb], writes=[h32b[r]])
                    P.op("pe", lambda e, kc=kc, r=r, lp=lp: e.matmul(
                        lp[0:NE, :], lhsT=wr_sb[:, kc, :], rhs=h32[r][:], start=(kc == 0), stop=(kc == KC - 1)),
                        reads=[h32b[r], wrb], writes=[lpb])
            if moe:
                P.op("dve", lambda e, th=th, lp=lp: e.tensor_copy(logT[:, 512 * th:512 * (th + 1)], lp[0:NE, :]),
                     reads=[lpb], writes=[lgb])
        if moe:
            lg = P.sb([128, NB, NE], F32)
            m8 = P.sb([128, NB, 8], F32)
            ex = P.sb([128, NB, NE], F32)
            msk = P.sb([128, NB, NE], F32)
            den = P.sb([128, NB], F32)
            nm = P.sb([128, NB], F32)
            for b in range(NB):
                pt, pb = bk.next(0, 6)
                P.op("pe", lambda e, pt=pt, b=b: e.transpose(pt[:, 0:NE], logT[:, 128 * b:128 * (b + 1)], ident[0:NE, 0:NE]),
                     reads=[lgb], writes=[pb])
                P.op("dve", lambda e, pt=pt, b=b: e.tensor_copy(lg[:, b, :], pt[:, 0:NE]), reads=[pb], writes=[gtb])
                P.op("dve", lambda e, b=b: e.max(m8[:, b, :], lg[:, b, :]), reads=[gtb], writes=[gtb])
                P.op("dve", lambda e, b=b: e.tensor_scalar(nm[:, b:b + 1], m8[:, b, 0:1], -1.0, None, ALU.mult),
                     reads=[gtb], writes=[gtb])
                P.op("act", lambda e, b=b: e.activation(ex[:, b, :], lg[:, b, :], AF.Exp, bias=nm[:, b:b + 1], scale=1.0),
                     reads=[gtb], writes=[gtb])
                P.op("dve", lambda e, b=b: e.tensor_scalar(msk[:, b, :], lg[:, b, :], m8[:, b, 1:2], None, ALU.is_ge),
                     reads=[gtb], writes=[gtb])
                P.op("dve", lambda e, b=b: e.tensor_tensor(ex[:, b, :], ex[:, b, :], msk[:, b, :], ALU.mult),
                     reads=[gtb], writes=[gtb])
                P.op("dve", lambda e, b=b: e.tensor_reduce(den[:, b:b + 1], ex[:, b, :], AX.X, ALU.add),
                     reads=[gtb], writes=[gtb])
                P.op("dve", lambda e, b=b: e.reciprocal(den[:, b:b + 1], den[:, b:b + 1]), reads=[gtb], writes=[gtb])
                P.op("dve", lambda e, b=b: e.tensor_scalar(gates[:, b, :], ex[:, b, :], den[:, b:b + 1], None, ALU.mult),
                     reads=[gtb], writes=[gtb])
        for b in range(NB):
            P.op("act", lambda e, b=b: e.activation(x_sb[:, b, :], x_sb[:, b, :], AF.Identity, scale=ALPHA),
                 reads=[xb[b]], writes=[xb[b]])
        ws = WStream(P, 3, KC, 512)
        hT = P.sb([128, 12, T], BF16, name="hT")
        hTb = P.buf()
        sg = [P.sb([128, 512], BF16) for _ in range(2)]
        sgb = P.bufs(2)
        tmp = [P.sb([128, 512], F32) for _ in range(2)]
        tmpb = P.bufs(2)
        n = 0
        n2 = 0
        for ei, (wg, wu, wd, dff) in enumerate(experts):
            ntile = (dff + 511) // 512
            tiles = [(512 * i, min(512, dff - 512 * i)) for i in range(ntile)]
            groups = []
            cur, cc = [], 0
            for tl in tiles:
                if cc + tl[1] // 128 > 12:
                    groups.append(cur)
                    cur, cc = [], 0
                cur.append(tl)
                cc += tl[1] // 128
            groups.append(cur)
            for grp in groups:
                g0 = grp[0][0]
                nch = sum(t_[1] for t_ in grp) // 128
                for (c0, cw) in grp:
                    wgt, wgb = ws.load(wg[:, c0:c0 + cw], KC, cw)
                    wut, wub = ws.load(wu[:, c0:c0 + cw], KC, cw)
                    for mi in range(cw // 128):
                        hc = (c0 - g0) // 128 + mi
                        for th in range(2):
                            ts_ = slice(512 * th, 512 * (th + 1))
                            pg, pgb = bk.next(0, 4)
                            pu, pub = bk.next(0, 4)
                            for k in range(KC):
                                mm(P, pg[:], pgb, wgt[:, k, 128 * mi:128 * (mi + 1)], hmT[:, k, ts_], [wgb, hb], k == 0, k == KC - 1)
                            for k in range(KC):
                                mm(P, pu[:], pub, wut[:, k, 128 * mi:128 * (mi + 1)], hmT[:, k, ts_], [wub, hb], k == 0, k == KC - 1)
                            r = n % 2
                            n += 1
                            P.op("act", lambda e, pg=pg, r=r: e.activation(sg[r][:], pg[:], AF.Silu), reads=[pgb], writes=[sgb[r]])
                            P.op("dve", lambda e, pu=pu, r=r, hc=hc, ts_=ts_: e.tensor_tensor(hT[:, hc, ts_], sg[r][:], pu[:], ALU.mult),
                                 reads=[sgb[r], pub], writes=[hTb])
                for ft in range(4):
                    fs = slice(512 * ft, 512 * (ft + 1))
                    wdt, wdb = ws.load(wd[g0:g0 + 128 * nch, fs], nch, 512)
                    for b in range(NB):
                        pt, pb = bk.next(4, 8)
                        for k in range(nch):
                            mm(P, pt[:], pb, hT[:, k, 128 * b:128 * (b + 1)], wdt[:, k, :], [wdb, hTb], k == 0, k == nch - 1)
                        r = n2 % 2
                        n2 += 1
                        P.op("dve", lambda e, pt=pt, r=r, fs=fs: e.tensor_tensor(tmp[r][:], pt[:], G[:, fs], ALU.mult),
                             reads=[pb], writes=[tmpb[r]])
                        if moe:
                            P.op("dve", lambda e, r=r, b=b, fs=fs, ei=ei: e.scalar_tensor_tensor(
                                out=x_sb[:, b, fs], in0=tmp[r][:], scalar=gates[:, b, ei:ei + 1], in1=x_sb[:, b, fs],
                                op0=ALU.mult, op1=ALU.add), reads=[tmpb[r], xb[b], gtb], writes=[xb[b]])
                        else:
                            P.op("dve", lambda e, r=r, b=b, fs=fs: e.tensor_tensor(x_sb[:, b, fs], x_sb[:, b, fs], tmp[r][:], ALU.add),
                                 reads=[tmpb[r], xb[b]], writes=[xb[b]])


def even_front(P, tag, xsrc, ident, consts, scc, shc, w_in, qg, kvg, pos_bc, lat, latb, extra=None):
    with P.stage(tag):
        hmT = P.sb([128, KC, T], BF16, name="hmT")
        hb = P.buf()
        ones_bf = P.sb([128, 128], BF16)
        ob = P.buf()
        P.op("dve", lambda e: e.memset(ones_bf[:], 1.0), writes=[ob])
        with P.stage(tag + "h"):
            x_sb = P.sb([128, NB, D], F32, name="x")
            xb = P.bufs(NB)
            chx = [P.chan() for _ in range(2)]
            bk = Banks(P, 6)
            for b in range(NB):
                P.dma("sp", chx[b % 2], x_sb[:, b, :], xsrc[128 * b:128 * (b + 1), :], writes=[xb[b]])
            for th in range(2):
                for kc in range(KC):
                    pt, pb = bk.next()
                    for i in range(4):
                        b = 4 * th + i
                        P.op("pe", lambda e, pt=pt, i=i, b=b, kc=kc: e.transpose(
                            pt[:, 128 * i:128 * (i + 1)], x_sb[:, b, 128 * kc:128 * (kc + 1)], ident[:]),
                            reads=[xb[b]], writes=[pb])
                    P.op("act", lambda e, pt=pt, kc=kc, th=th: e.activation(
                        hmT[:, kc, 512 * th:512 * (th + 1)], pt[:], AF.Identity,
                        bias=shc[:, kc:kc + 1], scale=scc[:, kc:kc + 1]), reads=[pb], writes=[hb])
        with P.stage(tag + "w"):
            bk = Banks(P, 8)
            ws = WStream(P, 2, KC, 512)
            sq = [P.sb([128, 512], BF16) for _ in range(2)]
            sqb = P.bufs(2)
            cos_t = P.sb([64, T], F32)
            sin_t = P.sb([64, T], F32)
            with P.stage(tag + "r"):
                tb = rope_tables(P, pos_bc, consts["rc"], cos_t, sin_t)
            if lat is not None:
                latent_part(P, bk, ws, w_in, hmT, hb, kvg, ones_bf, cos_t, sin_t, tb, lat, latb, sq, sqb)
            if extra is None:
                return
            X = extra
            ssbank = [bk.t[6], bk.t[7]]
            ssb = [bk.b[6], bk.b[7]]
            tiles = {}

            def wt_of(m):
                col = 128 * m
                t0 = col // 512
                if t0 not in tiles:
                    tiles[t0] = ws.load(w_in[:, 512 * t0:512 * t0 + 512], KC, 512)
                return tiles[t0][0], tiles[t0][1], col - 512 * t0
            rms_chunks(P, bk, wt_of, hmT, hb, None, 6, qg, X["cqg"], X["cqgb"], ones_bf, ssbank, ssb, sq, sqb)
            rstd_from_ss(P, ssbank, ssb, QR, X["rstdq"], X["rqb"], post=SM_SCALE)
            P.op("dve", lambda e: e.tensor_tensor(X["cq_t"][:], cos_t[:], X["rstdq"][0:64, :], ALU.mult),
                 reads=[tb, X["rqb"]], writes=[X["qtb"]])
            P.op("dve", lambda e: e.tensor_tensor(X["sq_t"][:], sin_t[:], X["rstdq"][0:64, :], ALU.mult),
                 reads=[tb, X["rqb"]], writes=[X["qtb"]])
            guT = P.sb([128, 8, T], BF16, name="guT")
            gub = P.buf()
            for t0 in range(2):
                wt, wb = ws.load(w_in[:, 1408 + 512 * t0:1408 + 512 * (t0 + 1)], KC, 512)
                for mi in range(4):
                    g = 4 * t0 + mi
                    for th in range(2):
                        pt, pb = bk.next(0, 4)
                        for k in range(KC):
                            mm(P, pt[:], pb, wt[:, k, 128 * mi:128 * (mi + 1)], hmT[:, k, 512 * th:512 * (th + 1)],
                               [wb, hb], k == 0, k == KC - 1)
                        P.op("act", lambda e, pt=pt, g=g, th=th: e.activation(
                            guT[:, g, 512 * th:512 * (th + 1)], pt[:], AF.Gelu_apprx_tanh), reads=[pb], writes=[gub])
            sguW = P.sb([128, 8, 128], BF16)
            swb = P.buf()
            chs = P.chan()
            P.dma("pool", chs, sguW[:], X["sgu_wT"], writes=[swb])
            for g in range(8):
                P.op("dve", lambda e, g=g: e.tensor_tensor(sguW[:, g, :], sguW[:, g, :], consts["triu"][:], ALU.mult),
                     reads=[swb], writes=[swb])
            sgub_sb = P.sb([1, 8 * 128], BF16)
            P.dma("pool", chs, sgub_sb[:], X["sgu_b"], writes=[swb])
            ng = P.sb([128, 1024], F32)
            nb_ = P.sb([128, 1024], F32)
            nbb = P.buf()
            P.dma("sp", chs, ng[:], X["sgu_ng"][0:1, :].to_broadcast([128, 1024]), writes=[nbb])
            P.dma("sp", chs, nb_[:], X["sgu_nb"][0:1, :].to_broadcast([128, 1024]), writes=[nbb])
            wv = [ws.load(w_in[:, 2432 + 512 * t0:2432 + 512 * (t0 + 1)], KC, 512) for t0 in range(2)]
            gv = P.sb([128, 1024], F32)
            gsq = P.sb([128, 1024], F32)
            vn = P.sb([128, 1024], F32)
            vgn = [P.sb([128, 1024], BF16) for _ in range(2)]
            vgb = P.bufs(2)
            gvb, stb = P.bufs(2)
            s1 = P.sb([128, 8], F32)
            s2 = P.sb([128, 8], F32)
            mean = P.sb([128, 8], F32)
            msq = P.sb([128, 8], F32)
            var = P.sb([128, 8], F32)
            rstd = P.sb([128, 8], F32)
            catT, catb = X["catT"], X["catb"]
            for b in range(NB):
                for t0 in range(2):
                    wt, wb = wv[t0]
                    pt, pb = bk.next(0, 4)
                    for k in range(KC):
                        mm(P, pt[:], pb, hmT[:, k, 128 * b:128 * (b + 1)], wt[:, k, :], [wb, hb], k == 0, k == KC - 1)
                    P.op("act", lambda e, pt=pt, t0=t0: e.activation(gv[:, 512 * t0:512 * (t0 + 1)], pt[:], AF.Gelu_apprx_tanh),
                         reads=[pb], writes=[gvb])
                P.op("act", lambda e: e.activation(gsq[:], gv[:], AF.Square), reads=[gvb], writes=[stb])
                P.op("dve", lambda e: e.tensor_reduce(s1[:], gv[:].rearrange("p (g c) -> p g c", g=8), AX.X, ALU.add),
                     reads=[gvb], writes=[stb])
                P.op("dve", lambda e: e.tensor_reduce(s2[:], gsq[:].rearrange("p (g c) -> p g c", g=8), AX.X, ALU.add),
                     reads=[stb], writes=[stb])
                P.op("dve", lambda e: e.tensor_scalar(mean[:], s1[:], 1.0 / 128, None, ALU.mult), reads=[stb], writes=[stb])
                P.op("dve", lambda e: e.tensor_tensor(msq[:], mean[:], mean[:], ALU.mult), reads=[stb], writes=[stb])
                P.op("dve", lambda e: e.scalar_tensor_tensor(out=var[:], in0=s2[:], scalar=1.0 / 128, in1=msq[:],
                                                             op0=ALU.mult, op1=ALU.subtract), reads=[stb], writes=[stb])
                rsqrt_op(P, rstd[:], var[:], 0, 1.0, [stb], [stb])
                for g in range(8):
                    gs = slice(128 * g, 128 * (g + 1))
                    P.op("dve", lambda e, g=g, gs=gs: e.tensor_scalar(vn[:, gs], gv[:, gs], mean[:, g:g + 1], rstd[:, g:g + 1],
                                                                      ALU.subtract, ALU.mult), reads=[stb, gvb], writes=[stb])
                r = b % 2
                P.op("dve", lambda e: e.tensor_tensor(vn[:], vn[:], ng[:], ALU.mult), reads=[stb, nbb], writes=[stb])
                P.op("dve", lambda e, r=r: e.tensor_tensor(vgn[r][:], vn[:], nb_[:], ALU.add), reads=[stb, nbb], writes=[vgb[r]])
                for g0 in range(0, 8, 4):
                    pt, pb = bk.next(4, 6)
                    for gi in range(4):
                        g = g0 + gi
                        mm(P, pt[:, 128 * gi:128 * (gi + 1)], pb, vgn[r][:, 128 * g:128 * (g + 1)], sguW[:, g, :],
                           [vgb[r], swb], True, False)
                        mm(P, pt[:, 128 * gi:128 * (gi + 1)], pb, ones_bf[0:1, :], sgub_sb[0:1, 128 * g:128 * (g + 1)],
                           [swb, ob], False, True)
                    P.op("dve", lambda e, pt=pt, g0=g0, b=b: e.tensor_tensor(
                        catT[:, 8 + g0:8 + g0 + 4, 128 * b:128 * (b + 1)], pt[:].rearrange("p (g t) -> p g t", g=4),
                        guT[:, g0:g0 + 4, 128 * b:128 * (b + 1)], ALU.mult), reads=[pb, gub], writes=[catb])


def q_up_stage(P, tag, wq, X):
    with P.stage(tag):
        bk = Banks(P, 8)
        ws = WStream(P, 2, 6, 512)
        t1 = [P.sb([64, 512], F32) for _ in range(2)]
        t2 = [P.sb([64, 512], F32) for _ in range(2)]
        t1b = P.bufs(2)
        t2b = P.bufs(2)
        n = 0
        for hp in range(4):
            wt, wb = ws.load(wq[:, 512 * hp:512 * (hp + 1)], 6, 512)
            for hi in range(2):
                h = 2 * hp + hi
                c0 = 256 * hi
                for th in range(2):
                    sl = slice(512 * th, 512 * (th + 1))
                    pn, pnb = bk.next()
                    pa, pab = bk.next()
                    pb_, pbb = bk.next()
                    for k in range(6):
                        mm(P, pn[:], pnb, wt[:, k, c0:c0 + 128], X["cqg"][:, k, sl], [wb, X["cqgb"]], k == 0, k == 5)
                    for k in range(6):
                        mm(P, pa[0:64, :], pab, wt[:, k, c0 + 128:c0 + 192], X["cqg"][:, k, sl], [wb, X["cqgb"]], k == 0, k == 5)
                    for k in range(6):
                        mm(P, pb_[0:64, :], pbb, wt[:, k, c0 + 192:c0 + 256], X["cqg"][:, k, sl], [wb, X["cqgb"]], k == 0, k == 5)
                    P.op("dve", lambda e, pn=pn, h=h, sl=sl: e.tensor_tensor(X["qn"][:, h, sl], pn[:], X["rstdq"][:, sl], ALU.mult),
                         reads=[pnb, X["rqb"]], writes=[X["qb"]])
                    r = n % 2
                    n += 1
                    P.op("dve", lambda e, pa=pa, r=r, sl=sl: e.tensor_tensor(t1[r][:], pa[0:64, :], X["cq_t"][:, sl], ALU.mult),
                         reads=[pab, X["qtb"]], writes=[t1b[r]])
                    P.op("dve", lambda e, pb_=pb_, r=r, sl=sl: e.tensor_tensor(t2[r][:], pb_[0:64, :], X["sq_t"][:, sl], ALU.mult),
                         reads=[pbb, X["qtb"]], writes=[t2b[r]])
                    P.op("dve", lambda e, r=r, h=h, sl=sl: e.tensor_tensor(X["qr"][:, h, sl], t1[r][:], t2[r][:], ALU.add),
                         reads=[t1b[r], t2b[r]], writes=[X["qb"]])


def attention_stage(P, tag, lat_all_d, wkv, amask_d, X):
    with P.stage(tag):
        bk = Banks(P, 8)
        lat = P.sb([128, 5, 4096], BF16, name="latall")
        lb = P.buf()
        chl = P.chan()
        for m in range(5):
            P.dma("pool", chl, lat[:, m, :], lat_all_d[m], writes=[lb])
        wk = P.sb([128, 4, 2048], BF16, name="wkv")
        wkb = P.buf()
        P.dma("pool", chl, wk[:], wkv.rearrange("(k p) n -> p k n", p=128), writes=[wkb])
        am = P.sb([128, NB, 4, 128], BF16, name="amask")
        amb = P.buf()
        P.dma("pool", chl, am[:], amask_d, writes=[amb])
        ones_f = P.sb([128, 128], F32)
        ofb = P.buf()
        P.op("dve", lambda e: e.memset(ones_f[:], 1.0), writes=[ofb])
        knT = P.sb([128, 4096], BF16, name="knT")
        V = P.sb([128, 32, 128], BF16, name="V")
        knb, vb = P.bufs(2)
        pT = [P.sb([128, 512], BF16) for _ in range(4)]
        pTb = P.bufs(4)
        dacc = P.sb([128, T], F32)
        dab = P.buf()
        rec = P.sb([128, T], F32)
        rcb = P.buf()
        catT, catb = X["catT"], X["catb"]
        npt = 0
        for h in range(NH):
            for kt in range(8):
                pt, pb = bk.next(0, 4)
                for k in range(4):
                    mm(P, pt[:], pb, wk[:, k, 256 * h:256 * h + 128], lat[:, k, 512 * kt:512 * (kt + 1)], [wkb, lb], k == 0, k == 3)
                P.op("act", lambda e, pt=pt, kt=kt: e.activation(knT[:, 512 * kt:512 * (kt + 1)], pt[:], AF.Identity),
                     reads=[pb], writes=[knb])
            for k4 in range(8):
                pt, pb = bk.next(0, 4)
                for i in range(4):
                    kb = 4 * k4 + i
                    for k in range(4):
                        mm(P, pt[:, 128 * i:128 * (i + 1)], pb, lat[:, k, 128 * kb:128 * (kb + 1)],
                           wk[:, k, 256 * h + 128:256 * h + 256], [wkb, lb], k == 0, k == 3)
                P.op("act", lambda e, pt=pt, k4=k4: e.activation(
                    V[:, 4 * k4:4 * k4 + 4, :], pt[:].rearrange("p (a d) -> p a d", a=4), AF.Identity), reads=[pb], writes=[vb])
            o_bank = [bk.t[6], bk.t[7]]
            o_b = [bk.b[6], bk.b[7]]
            for kb in range(32):
                i0 = kb // 4
                q0 = 128 * i0
                pieces = []
                s0 = q0
                while s0 < T:
                    e0 = min(T, (s0 // 512 + 1) * 512)
                    pieces.append((s0, e0))
                    s0 = e0
                for pi, (s0, e0) in enumerate(pieces):
                    nq = e0 - s0
                    half = s0 // 512
                    c0 = s0 - 512 * half
                    pt, pb = bk.next(0, 6) if False else bk.next(4, 6) if False else bk.next(0, 4)
                    mm(P, pt[:, 0:nq], pb, knT[:, 128 * kb:128 * (kb + 1)], X["qn"][:, h, s0:e0], [knb, X["qb"]], True, False)
                    mm(P, pt[:, 0:nq], pb, lat[0:64, 4, 128 * kb:128 * (kb + 1)], X["qr"][:, h, s0:e0], [lb, X["qb"]], False, True)
                    r = npt % 4
                    npt += 1
                    P.op("act", lambda e, pt=pt, r=r, nq=nq: e.activation(pT[r][:, 0:nq], pt[:, 0:nq], AF.Exp),
                         reads=[pb], writes=[pTb[r]])
                    if pi == 0:
                        P.op("dve", lambda e, r=r, i0=i0, kb=kb: e.tensor_tensor(pT[r][:, 0:128], pT[r][:, 0:128], am[:, i0, kb % 4, :], ALU.mult),
                             reads=[amb, pTb[r]], writes=[pTb[r]])
                    if kb == 0:
                        P.op("dve", lambda e, r=r, s0=s0, e0=e0, nq=nq: e.tensor_copy(dacc[:, s0:e0], pT[r][:, 0:nq]),
                             reads=[pTb[r]], writes=[dab])
                    else:
                        P.op("dve", lambda e, r=r, s0=s0, e0=e0, nq=nq: e.tensor_tensor(dacc[:, s0:e0], dacc[:, s0:e0], pT[r][:, 0:nq], ALU.add),
                             reads=[pTb[r]], writes=[dab])
                    last = (kb == 15 and half == 0) or (kb == 31)
                    P.op("pe", lambda e, r=r, half=half, c0=c0, nq=nq, kb=kb, last=last: e.matmul(
                        o_bank[half][:, c0:c0 + nq], lhsT=V[:, kb, :], rhs=pT[r][:, 0:nq], start=(kb == 0), stop=last,
                        skip_group_check=True), reads=[pTb[r], vb], writes=[o_b[half]])
            for half in range(2):
                sl = slice(512 * half, 512 * (half + 1))
                pt, pb = bk.next(4, 6)
                P.op("pe", lambda e, pt=pt, sl=sl: e.matmul(pt[:], lhsT=ones_f[:], rhs=dacc[:, sl], start=True, stop=True),
                     reads=[dab, ofb], writes=[pb])
                P.op("dve", lambda e, pt=pt, sl=sl: e.reciprocal(rec[:, sl], pt[:]), reads=[pb], writes=[rcb])
                P.op("dve", lambda e, half=half, sl=sl, h=h: e.tensor_tensor(catT[:, h, sl], o_bank[half][:], rec[:, sl], ALU.mult),
                     reads=[o_b[half], rcb], writes=[catb])


def pool_stage(P, tag, xsrc, xhalo, SC, SH, consts, pw, pscol, oT, ob):
    with P.stage(tag):
        bk = Banks(P, 8)
        dT = P.sb([128, KC, T], BF16, name="dT")
        db = P.buf()
        with P.stage(tag + "a"):
            xt = [P.sb([128, D], F32, name="xt") for _ in range(2)]
            xtb = P.bufs(2)
            xh = P.sb([96, 3, D], F32, name="xh")
            hm = P.sb([128, NB, D], BF16, name="hm")
            hh = P.sb([96, 3, D], BF16, name="hh")
            hmb = P.bufs(NB)
            xhb, hhb = P.bufs(2)
            chx = [P.chan() for _ in range(2)]
            chh = P.chan()
            P.dma("sp", chh, xh[:], xhalo.rearrange("s r f -> r s f"), writes=[xhb])
            for s_ in range(3):
                P.op("dve", lambda e, s_=s_: e.tensor_tensor(xh[:, s_, :], xh[:, s_, :], SC[0:96, :], ALU.mult), reads=[xhb], writes=[xhb])
                P.op("dve", lambda e, s_=s_: e.tensor_tensor(hh[:, s_, :], xh[:, s_, :], SH[0:96, :], ALU.add), reads=[xhb], writes=[hhb])
            for b in range(NB):
                r = b % 2
                P.dma("sp", chx[r], xt[r][:], xsrc[128 * b:128 * (b + 1), :], writes=[xtb[r]])
                P.op("dve", lambda e, r=r: e.tensor_tensor(xt[r][:], xt[r][:], SC[:], ALU.mult), reads=[xtb[r]], writes=[xtb[r]])
                P.op("dve", lambda e, b=b, r=r: e.tensor_tensor(hm[:, b, :], xt[r][:], SH[:], ALU.add), reads=[xtb[r]], writes=[hmb[b]])
            am, ah = consts["pool_am"], consts["pool_ah"]
            for kc in range(KC):
                w_i = kc // 4
                for th in range(2):
                    pt, pb = bk.next()
                    for i in range(4):
                        b = 4 * th + i
                        v = 0 if b == 0 else 1
                        mm(P, pt[:, 128 * i:128 * (i + 1)], pb, hm[:, b, 128 * kc:128 * (kc + 1)], am[:, v, w_i, :], [hmb[b]], True, False)
                        gq, sl_ = b % 3, b // 3
                        mm(P, pt[:, 128 * i:128 * (i + 1)], pb, hh[32 * gq:32 * gq + 32, sl_, 128 * kc:128 * (kc + 1)],
                           ah[32 * gq:32 * gq + 32, v, w_i, :], [hhb], False, True)
                    P.op("act", lambda e, pt=pt, kc=kc, th=th: e.activation(dT[:, kc, 512 * th:512 * (th + 1)], pt[:], AF.Identity),
                         reads=[pb], writes=[db])
        with P.stage(tag + "b"):
            ws = WStream(P, 2, 4, 512)
            for g in range(4):
                wt, wb = ws.load(pw[g], 4, 512)
                for co in range(4):
                    for th in range(2):
                        pt, pb = bk.next()
                        for k in range(4):
                            mm(P, pt[:], pb, wt[:, k, 128 * co:128 * (co + 1)], dT[:, 4 * g + k, 512 * th:512 * (th + 1)],
                               [wb, db], k == 0, k == 3)
                        c = 4 * g + co
                        P.op("dve", lambda e, pt=pt, c=c, th=th: e.tensor_scalar(
                            oT[:, c, 512 * th:512 * (th + 1)], pt[:], pscol[:, c:c + 1], None, ALU.mult), reads=[pb], writes=[ob])


def _din(nc, name, shape, dt=F32):
    return nc.dram_tensor(name, list(shape), dt, kind="ExternalInput").ap()


def _dout(nc, name, shape, dt=F32):
    return nc.dram_tensor(name, list(shape), dt, kind="ExternalOutput").ap()


def _load_const(P, ch, shape, dt, src, eng="sp"):
    t = P.sb(shape, dt)
    b = P.buf()
    P.dma(eng, ch, t[:], src, writes=[b])
    return t


def build(kind):
    nc = bass.Bass("TRN2", target_bir_lowering=False)
    x_in = _din(nc, "x_in", [T, D])
    ccol = _din(nc, "ccol", [128, KC])
    ident_d = _din(nc, "ident", [128, 128])
    ada = []
    for s in range(2):
        if kind == "lat" and s == 1:
            break
        ada.append((_din(nc, "adaw%d" % s, [D, 3 * D]), _din(nc, "adabr%d" % s, [1, 3 * D]), _din(nc, "adabc%d" % s, [128, 48])))
    with ExitStack() as st:
        P = Prog(nc, st)
        with P.stage("main"):
            chc = P.chan()
            ident = _load_const(P, chc, [128, 128], F32, ident_d)
            cv = P.sb([128, 4], F32, name="cv")
            cvb = P.buf()
            for ci, val in enumerate((LN_EPS, RMS_EPS, RMS_EPS / (SM_SCALE * SM_SCALE), 0.0)):
                P.op("dve", lambda e, ci=ci, val=val: e.memset(cv[:, ci:ci + 1], val), writes=[cvb])
            CV["t"] = cv
            P.flush()
            col = {i: P.sb([128, KC], F32) for i in range(3)}
            if kind in ("lat", "even"):
                w_in = _din(nc, "w_in", [D, W_IN_EXT])
                kvg_d = _din(nc, "kvg", [128, 4])
                qg_d = _din(nc, "qg", [128, 6])
                pos_d = _din(nc, "pos", [1, T], I32)
                rc_d = _din(nc, "rc", [64, 4])
                consts = {"rc": _load_const(P, chc, [64, 4], F32, rc_d)}
                kvg = _load_const(P, chc, [128, 4], F32, kvg_d)
                qg = _load_const(P, chc, [128, 6], F32, qg_d)
                pos_bc = pos_d[0:1, :].to_broadcast([64, T])
            if kind == "lat":
                lat_o = _dout(nc, "lat_o", [5, 128, T])
                stage_ada(P, "ada0", ccol, ada[0][0], ada[0][1], ada[0][2], (0, 1), (), col, {})
                lat = P.sb([128, 5, T], F32, name="lat")
                latb = P.buf()
                P.op("dve", lambda e: e.memset(lat[:, 4, :], 0.0), writes=[latb])
                even_front(P, "ef", x_in, ident, consts, col[1], col[0], w_in, qg, kvg, pos_bc, lat, latb, None)
                with P.stage("st"):
                    ch = P.chan()
                    for m in range(5):
                        P.dma("sp", ch, lat_o[m], lat[:, m, :], reads=[latb])
            elif kind == "even":
                lat_all = _din(nc, "lat_all", [5, 128, 4096])
                wq = _din(nc, "wq", [QR, 2048])
                wkv = _din(nc, "wkv", [KVR, 2048])
                amask = _din(nc, "amask", [128, NB, 4, 128])
                sgu_wT = _din(nc, "sgu_wT", [128, 8, 128])
                sgu_b = _din(nc, "sgu_b", [1, 1024])
                sgu_ng = _din(nc, "sgu_ng", [1, 1024])
                sgu_nb = _din(nc, "sgu_nb", [1, 1024])
                triu_d = _din(nc, "triu", [128, 128])
                w_out = _din(nc, "w_out", [D, D])
                lng0, lnb0 = _din(nc, "lng0", [1, D]), _din(nc, "lnb0", [1, D])
                lng1, lnb1 = _din(nc, "lng1", [1, D]), _din(nc, "lnb1", [1, D])
                wg, wu, wd = _din(nc, "ffn_wg", [D, DFF]), _din(nc, "ffn_wu", [D, DFF]), _din(nc, "ffn_wd", [DFF, D])
                x_mid = _dout(nc, "x_mid", [T, D])
                x_out = _dout(nc, "x_out", [T, D])
                consts["triu"] = _load_const(P, chc, [128, 128], BF16, triu_d, eng="pool")
                G = P.sb([128, D], F32, name="G")
                stage_ada(P, "ada0", ccol, ada[0][0], ada[0][1], ada[0][2], (0, 1), (2,), col, {2: G})
                with P.stage("mix"):
                    X = {}
                    X["catT"] = P.sb([128, KC, T], BF16, name="catT")
                    X["cqg"] = P.sb([128, 6, T], BF16, name="cqg")
                    X["rstdq"] = P.sb([128, T], F32)
                    X["cq_t"] = P.sb([64, T], F32)
                    X["sq_t"] = P.sb([64, T], F32)
                    for k in ("catb", "cqgb", "rqb", "qtb", "qb"):
                        X[k] = P.buf()
                    X.update(sgu_wT=sgu_wT, sgu_b=sgu_b, sgu_ng=sgu_ng, sgu_nb=sgu_nb)
                    even_front(P, "ef", x_in, ident, consts, col[1], col[0], w_in, qg, kvg, pos_bc, None, None, X)
                    with P.stage("att"):
                        X["qn"] = P.sb([128, NH, T], BF16, name="qn")
                        X["qr"] = P.sb([64, NH, T], BF16, name="qr")
                        q_up_stage(P, "qup", wq, X)
                        attention_stage(P, "attn", lat_all, wkv, amask, X)
                    out_proj_ln(P, "op", X["catT"], w_out, x_in, x_mid, G, lng0, lnb0)
                stage_ada(P, "ada1", ccol, ada[1][0], ada[1][1], ada[1][2], (0, 1), (2,), col, {2: G})
                ffn_stage(P, "ffn", x_mid, x_out, ident, col[1], col[0], G, lng1, lnb1, [(wg, wu, wd, DFF)])
            elif kind == "odd":
                xhalo = _din(nc, "xhalo", [3, 96, D])
                pam_d = _din(nc, "pool_am", [128, 2, 4, 128])
                pah_d = _din(nc, "pool_ah", [96, 2, 4, 128])
                pw = _din(nc, "pool_w", [4, 512, 512])
                psc_d = _din(nc, "pscol", [128, KC])
                w_out = _din(nc, "w_out", [D, D])
                lng0, lnb0 = _din(nc, "lng0", [1, D]), _din(nc, "lnb0", [1, D])
                lng1, lnb1 = _din(nc, "lng1", [1, D]), _din(nc, "lnb1", [1, D])
                wr = _din(nc, "router_w", [D, NE])
                mg, mu, md = _din(nc, "moe_wg", [NE, D, DFE]), _din(nc, "moe_wu", [NE, D, DFE]), _din(nc, "moe_wd", [NE, DFE, D])
                x_mid = _dout(nc, "x_mid", [T, D])
                x_out = _dout(nc, "x_out", [T, D])
                consts = {"pool_am": _load_const(P, chc, [128, 2, 4, 128], BF16, pam_d, eng="pool"),
                          "pool_ah": _load_const(P, chc, [96, 2, 4, 128], BF16, pah_d, eng="pool")}
                pscol = _load_const(P, chc, [128, KC], F32, psc_d)
                G = P.sb([128, D], F32, name="G")
                with P.stage("mix"):
                    oT = P.sb([128, KC, T], BF16, name="oT")
                    ob = P.buf()
                    with P.stage("mix2"):
                        SC = P.sb([128, D], F32, name="SC")
                        SH = P.sb([128, D], F32, name="SH")
                        stage_ada(P, "ada0", ccol, ada[0][0], ada[0][1], ada[0][2], (), (0, 1, 2), {}, {0: SH, 1: SC, 2: G})
                        pool_stage(P, "pool", x_in, xhalo, SC, SH, consts, pw, pscol, oT, ob)
                    out_proj_ln(P, "op", oT, w_out, x_in, x_mid, G, lng0, lnb0)
                stage_ada(P, "ada1", ccol, ada[1][0], ada[1][1], ada[1][2], (0, 1), (2,), col, {2: G})
                ffn_stage(P, "moe", x_mid, x_out, ident, col[1], col[0], G, lng1, lnb1,
                          [(mg[e], mu[e], md[e], DFE) for e in range(NE)], router_w=wr)
    return nc


NCORES = 8
START_LAYER = 0
_PROGS = {}


def _prog(kind):
    if kind not in _PROGS:
        _PROGS[kind] = build(kind)
    return _PROGS[kind]


def core_blocks(j):
    return [j, 7 - j, 8 + j, 15 - j, 16 + j, 23 - j, 24 + j, 31 - j]


def core_tok_idx(j):
    return np.concatenate([128 * g + np.arange(128) for g in core_blocks(j)])


def _f32(a):
    return np.ascontiguousarray(a, dtype=np.float32)


def _col(v, n):
    return _f32(np.asarray(v).reshape(n, 128).T)


def _consts():
    c = {}
    c["ident"] = np.eye(128, dtype=np.float32)
    inv = (10000.0 ** (-np.arange(0, 64, 2, dtype=np.float32) / 64)).astype(np.float32)
    rc = np.zeros((64, 4), np.float32)
    rc[:, 0] = np.concatenate([inv, inv])
    rc[:, 1] = 0.5 * math.pi
    rc[:, 2] = np.concatenate([np.full(32, math.pi), np.zeros(32)])
    c["rc"] = rc
    s_ = np.arange(128)
    c["triu"] = (s_[:, None] <= s_[None, :]).astype(np.float32)
    return c


def _pool_consts(j):
    wins = (2, 4, 8, 16)
    am = np.zeros((128, 2, 4, 128), np.float32)
    ah = np.zeros((96, 2, 4, 128), np.float32)
    for wi, w in enumerate(wins):
        A = np.zeros((128, 128), np.float32)
        A0 = np.zeros((128, 128), np.float32)
        H = np.zeros((128, 16), np.float32)
        for t in range(128):
            for jj in range(t - w + 1, t + 1):
                if jj >= 0:
                    A[t, jj] += 1.0 / w
                    A0[t, jj] += 1.0 / min(t + 1, w)
                else:
                    H[t, 16 + jj] += 1.0 / w
            A[t, t] -= 1.0
            A0[t, t] -= 1.0
        first = (j == 0)
        am[:, 0, wi, :] = (A0 if first else A).T
        am[:, 1, wi, :] = A.T
        for gq in range(3):
            ah[32 * gq:32 * gq + 16, 1, wi, :] = H.T
            if not first:
                ah[32 * gq:32 * gq + 16, 0, wi, :] = H.T
    return am, ah


def _amask(j):
    blocks = core_blocks(j)
    m = np.zeros((128, NB, 4, 128), np.float32)
    s_ = np.arange(128)
    tri = (s_[:, None] <= s_[None, :]).astype(np.float32)
    for i, g in enumerate(blocks):
        for r in range(4):
            kb = 4 * i + r
            if kb < g:
                m[:, i, r, :] = 1.0
            elif kb == g:
                m[:, i, r, :] = tri
    return m


def _run(kind, in_maps):
    res = run_bass_kernel_spmd(_prog(kind), in_maps, core_ids=list(range(NCORES)))
    return res.results


def _ada_in(ada_w, ada_b, l, s):
    return {"adaw%d" % s: _f32(ada_w[l, s]), "adabr%d" % s: _f32(ada_b[l, s][None, :]),
            "adabc%d" % s: _col(ada_b[l, s], 48)}


def _w_in_ext(w):
    kr = w[:, 1280:1344]
    krs = np.concatenate([kr[:, 32:], kr[:, :32]], axis=1)
    return _f32(np.concatenate([w[:, :1344], krs, w[:, 1344:]], axis=1))


def _wq_ext(w):
    w = w.reshape(QR, NH, 192)
    rope = w[:, :, 128:]
    rs = np.concatenate([rope[:, :, 32:], rope[:, :, :32]], axis=2)
    return _f32(np.concatenate([w, rs], axis=2).reshape(QR, NH * 256))


def _wkv_ext(w):
    return _f32(w)


def kernel(x, c, positions, ada_w, ada_b, ln_g, ln_b, w_in_ab, q_norm_g, w_q_up, kv_norm_g, w_kv_up,
           sgu_norm_g, sgu_norm_b, sgu_w, sgu_b, w_out_ab, ffn_w_gate, ffn_w_up, ffn_w_down,
           pool_w, pool_scale, w_out_c, router_w, moe_w_gate, moe_w_up, moe_w_down, _debug=None):
    x = np.asarray(x, dtype=np.float32)
    cst = _consts()
    idx = [core_tok_idx(cid % 4) for cid in range(NCORES)]
    bat = [cid // 4 for cid in range(NCORES)]
    xs = [np.ascontiguousarray(x[bat[k], idx[k]]) for k in range(NCORES)]
    ccols = [_col(np.asarray(c)[bat[k]], KC) for k in range(NCORES)]
    poss = [np.ascontiguousarray(np.asarray(positions)[bat[k], idx[k]][None, :].astype(np.int32)) for k in range(NCORES)]
    amasks = [_amask(k % 4) for k in range(NCORES)]
    pconsts = [_pool_consts(k % 4) for k in range(NCORES)]
    dbg = {} if _debug is not None else None
    for l in range(START_LAYER, DEPTH):
        j = l // 2
        if l % 2 == 0:
            w_in = _w_in_ext(np.asarray(w_in_ab[j]))
            common = {"ident": cst["ident"], "w_in": w_in, "kvg": _col(kv_norm_g[j], 4), "qg": _col(q_norm_g[j], 6),
                      "rc": cst["rc"]}
            common.update(_ada_in(ada_w, ada_b, l, 0))
            maps = [dict(common, x_in=xs[k], ccol=ccols[k], pos=poss[k]) for k in range(NCORES)]
            r = _run("lat", maps)
            lat_all = [np.zeros((5, 128, 4096), np.float32) for _ in range(2)]
            for k in range(NCORES):
                lat_all[bat[k]][:, :, idx[k]] = r[k]["lat_o"]
            if dbg is not None:
                dbg["lat%d" % l] = lat_all
                if _debug == "lat":
                    return dbg
            common.update(_ada_in(ada_w, ada_b, l, 1))
            common.update({
                "wq": _wq_ext(np.asarray(w_q_up[j])), "wkv": _wkv_ext(np.asarray(w_kv_up[j])),
                "sgu_wT": _f32(np.transpose(np.asarray(sgu_w[j]), (2, 0, 1))),
                "sgu_b": _f32(np.asarray(sgu_b[j]).reshape(1, 1024)),
                "sgu_ng": _f32(np.asarray(sgu_norm_g[j])[None, :]), "sgu_nb": _f32(np.asarray(sgu_norm_b[j])[None, :]),
                "triu": cst["triu"], "w_out": _f32(w_out_ab[j]),
                "lng0": _f32(ln_g[l, 0][None, :]), "lnb0": _f32(ln_b[l, 0][None, :]),
                "lng1": _f32(ln_g[l, 1][None, :]), "lnb1": _f32(ln_b[l, 1][None, :]),
                "ffn_wg": _f32(ffn_w_gate[j]), "ffn_wu": _f32(ffn_w_up[j]), "ffn_wd": _f32(ffn_w_down[j])})
            maps = [dict(common, x_in=xs[k], ccol=ccols[k], pos=poss[k], lat_all=lat_all[bat[k]], amask=amasks[k])
                    for k in range(NCORES)]
            r = _run("even", maps)
        else:
            xfull = np.zeros((2, 4096, D), np.float32)
            for k in range(NCORES):
                xfull[bat[k], idx[k]] = xs[k]
            common = {"ident": cst["ident"], "pool_w": _f32(pool_w[j]), "pscol": _col(pool_scale[j], KC),
                      "w_out": _f32(w_out_c[j]),
                      "lng0": _f32(ln_g[l, 0][None, :]), "lnb0": _f32(ln_b[l, 0][None, :]),
                      "lng1": _f32(ln_g[l, 1][None, :]), "lnb1": _f32(ln_b[l, 1][None, :]),
                      "router_w": _f32(router_w[j]), "moe_wg": _f32(moe_w_gate[j]), "moe_wu": _f32(moe_w_up[j]),
                      "moe_wd": _f32(moe_w_down[j])}
            common.update(_ada_in(ada_w, ada_b, l, 0))
            common.update(_ada_in(ada_w, ada_b, l, 1))
            maps = []
            for k in range(NCORES):
                xh = np.zeros((3, 96, D), np.float32)
                for b, g in enumerate(core_blocks(k % 4)):
                    if g > 0:
                        xh[b // 3, 32 * (b % 3):32 * (b % 3) + 16] = xfull[bat[k], 128 * g - 16:128 * g]
                maps.append(dict(common, x_in=xs[k], ccol=ccols[k], xhalo=xh, pool_am=pconsts[k][0], pool_ah=pconsts[k][1]))
            r = _run("odd", maps)
        if dbg is not None:
            for nm in ("x_mid", "x_out"):
                full = np.zeros((2, 4096, D), np.float32)
                for k in range(NCORES):
                    full[bat[k], idx[k]] = r[k][nm]
                dbg["%s%d" % (nm, l)] = full
            if _debug == l:
                return dbg
        xs = [np.ascontiguousarray(r[k]["x_out"]) for k in range(NCORES)]
    out = np.zeros((2, 4096, D), np.float32)
    for k in range(NCORES):
        out[bat[k], idx[k]] = xs[k]
    return out
```
